# Optimizing a Trainium2 kernel written in Bass

```python
import jax, jax.numpy as jnp
from jax import lax
import numpy as np

D_MODEL = 1024
BATCH = 4
SEQ = 4096
DEPTH = 2

N_MIXERS = 2
CONV_WIDTH = 31
HGRN_HEADS = 8
HGRN_KEY_DIM = D_MODEL // HGRN_HEADS
HGRN_VAL_DIM = D_MODEL // HGRN_HEADS
CHUNK = 64
N_GROUPS = 4
EXPERTS_PER_GROUP = 8
N_EXPERTS = N_GROUPS * EXPERTS_PER_GROUP
TOP_K_INNER = 2
D_EXPERT = 512
MOE_BLOCK = 128
NORM_EPS = 1e-6

kernel_name = 'hybrid_conformer_hgrn2_hmoe'


def rms_norm(x, w):
    xf = x.astype(jnp.float32)
    y = xf * lax.rsqrt(jnp.mean(xf * xf, axis=-1, keepdims=True) + NORM_EPS)
    return (y * w.astype(jnp.float32)).astype(x.dtype)


def conformer_conv(h, pw1_w, pw1_b, dw_w, dw_b, ln_g, ln_b, pw2_w, pw2_b):
    a = h @ pw1_w + pw1_b
    u = a[..., :D_MODEL] * jax.nn.sigmoid(a[..., D_MODEL:])
    c = lax.conv_general_dilated(
        u, dw_w[:, None, :].astype(u.dtype), window_strides=(1,),
        padding=[(CONV_WIDTH - 1, 0)], dimension_numbers=('NWC', 'WIO', 'NWC'),
        feature_group_count=D_MODEL) + dw_b
    cf = c.astype(jnp.float32)
    mu = jnp.mean(cf, axis=-1, keepdims=True)
    var = jnp.mean(jnp.square(cf - mu), axis=-1, keepdims=True)
    n = ((cf - mu) * lax.rsqrt(var + NORM_EPS) * ln_g.astype(jnp.float32) + ln_b.astype(jnp.float32)).astype(h.dtype)
    return jax.nn.silu(n) @ pw2_w + pw2_b


def hgrn2(h, w_in, gnorm_w, w_out, lb):
    B, S, _ = h.shape
    nc = S // CHUNK
    proj = h @ w_in
    q, f, i, g = jnp.split(proj, 4, axis=-1)
    q = jax.nn.silu(q.astype(jnp.float32))
    lbf = lb.astype(jnp.float32)
    forget = lbf + (1.0 - lbf) * jax.nn.sigmoid(f.astype(jnp.float32))
    k = 1.0 - forget
    log_f = jnp.log(forget)

    def to_chunks(t, dh):
        return t.astype(jnp.float32).reshape(B, nc, CHUNK, HGRN_HEADS, dh).transpose(1, 0, 3, 2, 4)

    qc = to_chunks(q, HGRN_KEY_DIM)
    kc = to_chunks(k, HGRN_KEY_DIM)
    vc = to_chunks(i, HGRN_VAL_DIM)
    Gc = jnp.cumsum(to_chunks(log_f, HGRN_KEY_DIM), axis=3)
    causal = jnp.tril(jnp.ones((CHUNK, CHUNK), dtype=bool))

    def step(state, inp):
        qb, kb, vb, Gb = inp
        inter = jnp.einsum('bhtk,bhkv->bhtv', qb * jnp.exp(Gb), state)
        diff = Gb[:, :, :, None, :] - Gb[:, :, None, :, :]
        decay = jnp.exp(jnp.where(causal[:, :, None], diff, -jnp.inf))
        scores = jnp.einsum('bhtsk,bhsk->bhts', qb[:, :, :, None, :] * decay, kb)
        intra = jnp.einsum('bhts,bhsv->bhtv', scores, vb)
        G_last = Gb[:, :, -1, :]
        new_state = jnp.exp(G_last)[..., None] * state + jnp.einsum(
            'bhsk,bhsv->bhkv', kb * jnp.exp(G_last[:, :, None, :] - Gb), vb)
        return new_state, inter + intra

    init = jnp.zeros((B, HGRN_HEADS, HGRN_KEY_DIM, HGRN_VAL_DIM), jnp.float32)
    _, o = lax.scan(step, init, (qc, kc, vc, Gc))
    o = o.transpose(1, 0, 3, 2, 4).reshape(B, S, HGRN_HEADS, HGRN_VAL_DIM)
    o = o * lax.rsqrt(jnp.mean(o * o, axis=-1, keepdims=True) + NORM_EPS) * gnorm_w.astype(jnp.float32)
    o = o * jax.nn.silu(g.astype(jnp.float32).reshape(B, S, HGRN_HEADS, HGRN_VAL_DIM))
    return o.reshape(B, S, D_MODEL).astype(h.dtype) @ w_out


def hier_moe(h, grp_w, grp_b, exp_w, exp_b, w_gate, w_up, w_down):
    B, S, D = h.shape
    T = B * S
    hf = h.reshape(T, D)
    grp_prob = jax.nn.softmax((hf @ grp_w + grp_b).astype(jnp.float32), axis=-1)
    grp_val, grp_idx = lax.top_k(grp_prob, 1)
    exp_logits = jnp.einsum('td,dge->tge', hf, exp_w) + exp_b
    in_grp = jnp.take_along_axis(exp_logits, grp_idx[:, :, None], axis=1)[:, 0].astype(jnp.float32)
    top_val, top_idx = lax.top_k(in_grp, TOP_K_INNER)
    gate = (grp_val * jax.nn.softmax(top_val, axis=-1)).reshape(-1)
    expert = (grp_idx * EXPERTS_PER_GROUP + top_idx).reshape(-1)
    token = jnp.repeat(jnp.arange(T, dtype=jnp.int32), TOP_K_INNER)

    onehot = jax.nn.one_hot(expert, N_EXPERTS, dtype=jnp.int32)
    counts = jnp.sum(onehot, axis=0)
    rank = jnp.take_along_axis(jnp.cumsum(onehot, axis=0), expert[:, None], axis=1)[:, 0] - 1
    padded = (counts + MOE_BLOCK - 1) // MOE_BLOCK * MOE_BLOCK
    pad_end = jnp.cumsum(padded)
    pad_start = pad_end - padded
    dest = pad_start[expert] + rank
    n_rows = T * TOP_K_INNER + N_EXPERTS * MOE_BLOCK
    n_blocks = n_rows // MOE_BLOCK
    x_pad = jnp.zeros((n_rows, D), h.dtype).at[dest].set(hf[token])
    tok_pad = jnp.zeros((n_rows,), jnp.int32).at[dest].set(token)
    gate_pad = jnp.zeros((n_rows,), jnp.float32).at[dest].set(gate)
    block_expert = jnp.minimum(
        jnp.searchsorted(pad_end, jnp.arange(n_blocks, dtype=jnp.int32) * MOE_BLOCK, side='right'),
        N_EXPERTS - 1)

    def expert_block(args):
        xb, e = args
        return (jax.nn.silu(xb @ w_gate[e]) * (xb @ w_up[e])) @ w_down[e]

    y = lax.map(expert_block, (x_pad.reshape(n_blocks, MOE_BLOCK, D), block_expert)).reshape(n_rows, D)
    y = (y.astype(jnp.float32) * gate_pad[:, None]).astype(h.dtype)
    return jnp.zeros((T, D), h.dtype).at[tok_pad].add(y).reshape(B, S, D)


def setup_inputs(seed: int = 0) -> dict:
    key = jax.random.key(seed)
    keys = iter(jax.random.split(key, 32))
    n_conv = (DEPTH + 1) // 2
    n_hgrn = DEPTH // 2
    D = D_MODEL

    def nrm(shape, scale):
        return jax.random.normal(next(keys), shape, jnp.float32) * scale

    def gain(shape):
        return 1.0 + nrm(shape, 0.02)

    return {
        'x': nrm((BATCH, SEQ, D), 1.0),
        'conv_norm_w': gain((n_conv, D)),
        'conv_pw1_w': nrm((n_conv, D, 2 * D), D ** -0.5),
        'conv_pw1_b': nrm((n_conv, 2 * D), 0.02),
        'conv_dw_w': nrm((n_conv, CONV_WIDTH, D), CONV_WIDTH ** -0.5),
        'conv_dw_b': nrm((n_conv, D), 0.02),
        'conv_ln_g': gain((n_conv, D)),
        'conv_ln_b': nrm((n_conv, D), 0.02),
        'conv_pw2_w': nrm((n_conv, D, D), D ** -0.5),
        'conv_pw2_b': nrm((n_conv, D), 0.02),
        'hgrn_norm_w': gain((n_hgrn, D)),
        'hgrn_w_in': nrm((n_hgrn, D, 4 * D), D ** -0.5),
        'hgrn_gnorm_w': gain((n_hgrn, HGRN_VAL_DIM)),
        'hgrn_w_out': nrm((n_hgrn, D, D), D ** -0.5),
        'lower_bounds': nrm((DEPTH, D), 0.5),
        'ffn_norm_w': gain((DEPTH, D)),
        'router_grp_w': nrm((DEPTH, D, N_GROUPS), D ** -0.5),
        'router_grp_b': nrm((DEPTH, N_GROUPS), 0.01),
        'router_exp_w': nrm((DEPTH, D, N_GROUPS, EXPERTS_PER_GROUP), D ** -0.5),
        'router_exp_b': nrm((DEPTH, N_GROUPS, EXPERTS_PER_GROUP), 0.01),
        'moe_w_gate': nrm((DEPTH, N_EXPERTS, D, D_EXPERT), D ** -0.5),
        'moe_w_up': nrm((DEPTH, N_EXPERTS, D, D_EXPERT), D ** -0.5),
        'moe_w_down': nrm((DEPTH, N_EXPERTS, D_EXPERT, D), D_EXPERT ** -0.5),
        'final_norm_w': gain((D,)),
    }


def reference(x, conv_norm_w, conv_pw1_w, conv_pw1_b, conv_dw_w, conv_dw_b, conv_ln_g, conv_ln_b,
              conv_pw2_w, conv_pw2_b, hgrn_norm_w, hgrn_w_in, hgrn_gnorm_w, hgrn_w_out, lower_bounds,
              ffn_norm_w, router_grp_w, router_grp_b, router_exp_w, router_exp_b,
              moe_w_gate, moe_w_up, moe_w_down, final_norm_w):
    lb_p = jax.nn.softmax(lower_bounds.astype(jnp.float32), axis=0)
    lb_all = jnp.cumsum(lb_p, axis=0) - lb_p[0]
    h = x
    for layer in range(DEPTH):
        j = layer // N_MIXERS
        if layer % N_MIXERS == 0:
            h = h + conformer_conv(rms_norm(h, conv_norm_w[j]), conv_pw1_w[j], conv_pw1_b[j],
                                   conv_dw_w[j], conv_dw_b[j], conv_ln_g[j], conv_ln_b[j],
                                   conv_pw2_w[j], conv_pw2_b[j])
        else:
            h = h + hgrn2(rms_norm(h, hgrn_norm_w[j]), hgrn_w_in[j], hgrn_gnorm_w[j],
                          hgrn_w_out[j], lb_all[layer])
        h = h + hier_moe(rms_norm(h, ffn_norm_w[layer]), router_grp_w[layer], router_grp_b[layer],
                         router_exp_w[layer], router_exp_b[layer], moe_w_gate[layer],
                         moe_w_up[layer], moe_w_down[layer])
    return rms_norm(h, final_norm_w)
```

```python
from contextlib import ExitStack
import numpy as np
import concourse.bass as bass
import concourse.mybir as mybir
from concourse.bass_utils import run_bass_kernel_spmd

F32 = mybir.dt.float32
BF16 = mybir.dt.bfloat16
I32 = mybir.dt.int32
AF = mybir.ActivationFunctionType
ALU = mybir.AluOpType
AX = mybir.AxisListType

P = 128
D = 1024
NT = 16
TOK = NT * P
NCORES = 8
CAP = 512
NE = 32
NSLOT = NE * CAP
EPS = 1e-6
HAL = 32
TALL = HAL + TOK


class Eng:
    def __init__(self, name, sem):
        self.name, self.sem, self.n, self.waited, self.plan = name, sem, 0, {}, []

    def wait(self, *toks):
        for t in toks:
            if t is None:
                continue
            if isinstance(t, (list, tuple)) and (len(t) == 0 or not isinstance(t[0], (Eng, DmaSem))):
                self.wait(*t)
                continue
            src, v = t
            if self.waited.get(id(src), 0) >= v:
                continue
            self.waited[id(src)] = v
            self.plan.append(lambda e, sem=src.sem, v=v: e.wait_ge(sem, v))

    def do(self, fn, deps=()):
        self.wait(*deps)
        self.n += 1
        self.plan.append(lambda e, fn=fn, sem=self.sem: fn(e).then_inc(sem, 1))
        return (self, self.n)

    def dma(self, dsem, out, in_, deps=(), **kw):
        self.wait(*deps)
        dsem.n += 16
        self.plan.append(lambda e, out=out, in_=in_, sem=dsem.sem, kw=kw: e.dma_start(out=out, in_=in_, **kw).then_inc(sem, 16))
        return (dsem, dsem.n)

    def raw16(self, dsem, fn, deps=()):
        self.wait(*deps)
        dsem.n += 16
        self.plan.append(lambda e, fn=fn, sem=dsem.sem: fn(e).then_inc(sem, 16))
        return (dsem, dsem.n)

    def replay(self, e):
        for p in self.plan:
            p(e)


class DmaSem:
    def __init__(self, sem):
        self.sem, self.n = sem, 0


class K:
    def __init__(self, stages):
        self.stages = stages
        self.nc = bass.Bass("TRN2", target_bir_lowering=False)
        self.es = ExitStack()
        nc = self.nc
        self.inp = {}
        self._uid = 0

    def din(self, name, shape, dt=F32):
        t = self.nc.dram_tensor(name, list(shape), dt, kind="ExternalInput").ap()
        self.inp[name] = t
        return t

    def sb(self, name, shape, dt, es=None):
        self._uid += 1
        return (es or self.es).enter_context(self.nc.sbuf_tensor("%s_s%d" % (name, self._uid), list(shape), dt))

    def ps(self, name, shape, dt, es=None):
        return (es or self.es).enter_context(self.nc.psum_tensor(name, list(shape), dt))

    def sem(self, name):
        return self.es.enter_context(self.nc.semaphore(name))

    def dsem(self, name):
        return DmaSem(self.sem(name))

    def barrier(self):
        toks = [(e, e.n) for e in self.engs if e.n > 0] + [(d, d.n) for d in self.dsems if d.n > 0]
        for e in self.engs:
            e.wait(*toks)

    def build(self):
        nc = self.nc
        st = self.stages
        x_c = self.din("x_c", [TOK, D])
        x_h = self.din("x_h", [P, D])
        cols = self.din("cols", [P, 320])
        rows = self.din("rows", [8, D])
        ident_d = self.din("ident", [P, P])
        ltri_d = self.din("ltri", [P, P])
        ebase_d = self.din("ebase", [P, NE])
        w1 = self.din("conv_pw1_w", [D, 2 * D])
        w2 = self.din("conv_pw2_w", [D, D])
        b2row = self.din("conv_pw2_b", [1, D])
        wr = self.din("wr", [2, D, 36])
        rbias = self.din("rbias", [2, 36])
        self.moe_layers = [l for l in (0, 1) if ("moe%d" % l) in st]
        NL = max(1, len(self.moe_layers))
        wg = self.din("moe_w_gate", [NL, NE, D, 512])
        wu = self.din("moe_w_up", [NL, NE, D, 512])
        wd = self.din("moe_w_down", [NL, NE, 512, D])
        w_in = self.din("hgrn_w_in", [D, 4 * D])
        w_out = self.din("hgrn_w_out", [D, D])
        bdle_d = self.din("bdle", [P, P])
        bu_d = self.din("bu", [P, P])
        s_init = self.din("s_init", [8, P, P])
        out_d = nc.dram_tensor("out", [TOK, D], F32, kind="ExternalOutput").ap()
        s_end = nc.dram_tensor("s_end", [8, P, P], F32, kind="ExternalOutput").ap()
        Xg = nc.dram_tensor("Xg", [NSLOT, D], BF16).ap()
        Yg = nc.dram_tensor("Yg", [NSLOT + 8, D], F32).ap()

        self.act = act = Eng("act", self.sem("s_act"))
        self.dve = dve = Eng("dve", self.sem("s_dve"))
        self.pe = pe = Eng("pe", self.sem("s_pe"))
        self.pool = pool = Eng("pool", self.sem("s_pool"))
        self.sp = sp = Eng("sp", self.sem("s_sp"))
        self.engs = [act, dve, pe, pool, sp]
        self.dsems = []

        def DS(name):
            d = self.dsem(name)
            self.dsems.append(d)
            return d

        d_c = DS("d_const"); d_x = DS("d_x"); d_w = [DS("d_w0"), DS("d_w1")]; d_o = DS("d_o")
        d_sc = DS("d_sc"); d_ga = [DS("d_ga%d" % i) for i in range(4)]; d_xg = [DS("d_xg0"), DS("d_xg1")]
        d_y = [DS("d_y0"), DS("d_y1")]
        d_wh = [DS("d_wh0"), DS("d_wh1")]; d_s = DS("d_s")

        h = self.sb("h", [P, NT, D], F32)
        ident = self.sb("identf", [P, P], F32)
        identb = self.sb("identb", [P, P], BF16)
        colv = self.sb("colv", [P, 320], F32)
        wbc = self.sb("wbc", [P, D], F32)
        ssq = self.sb("ssq", [P, NT + 1], F32)
        rstd = self.sb("rstd", [P, NT + 1], F32)
        junk = self.sb("junk", [P, D], BF16)
        onesb = self.sb("onesb", [P, P], BF16)
        onesm = self.sb("onesm", [P, P], F32)
        pb = [self.ps("pb%d" % i, [P, 512], F32) for i in range(7)]
        pbb = self.ps("pbb", [P, 1024], BF16)

        t_c = [sp.dma(d_c, ident[:], ident_d[:, :]), sp.dma(d_c, colv[:], cols[:, :])]
        t_idb = dve.do(lambda e: e.tensor_copy(out=identb[:], in_=ident[:]), [t_c])
        t_o1 = dve.do(lambda e: e.memset(onesb[:], 1.0))
        t_o2 = dve.do(lambda e: e.memset(onesm[:], 1.0 / D))
        t_x = None
        for i in range(NT):
            t_x = sp.dma(d_x, h[:, i, :], x_c[i * P:(i + 1) * P, :])

        C_B1 = 0
        C_DWB = 16
        C_LNG = 24
        C_LNB = 32
        C_DWW = 40
        C_HM = 288
        C_LB0 = 289
        C_LB1 = 297
        C_GN = 305
        R_CONVN, R_FFN0, R_FFN1, R_HGN, R_FIN = 0, 1, 2, 3, 4

        def rms_stats(ntile, src_of, deps):
            t = None
            for i in range(ntile):
                t = act.do(lambda e, i=i: e.activation(out=junk[:], in_=src_of(i), func=AF.Square, accum_out=ssq[:, i:i + 1]), deps)
            t1 = dve.do(lambda e: e.tensor_scalar(out=rstd[:, 0:ntile], in0=ssq[:, 0:ntile], scalar1=1.0 / D, scalar2=EPS, op0=ALU.mult, op1=ALU.add), [t])
            t2 = act.do(lambda e: e.activation(out=rstd[:, 0:ntile], in_=rstd[:, 0:ntile], func=AF.Sqrt), [t1])
            t3 = dve.do(lambda e: e.reciprocal(out=rstd[:, 0:ntile], in_=rstd[:, 0:ntile]), [t2])
            return t3

        if "conv" in st:
            with ExitStack() as es1:
                uT = self.sb("uT", [P, 8, TALL], BF16, es1)
                t_wbc = sp.dma(d_c, wbc[:], rows[R_CONVN:R_CONVN + 1, :].partition_broadcast(P))
                with ExitStack() as es2:
                    xT = self.sb("xT", [P, 8, TALL], BF16, es2)
                    w1s = self.sb("w1s", [P, 8, 2 * D], BF16, es2)
                    xh = self.sb("xh", [P, D], F32, es2)
                    xs = [self.sb("xs%d" % i, [P, D], F32, es2) for i in range(2)]
                    sg = [self.sb("sg%d" % i, [P, 416], F32, es2) for i in range(2)]
                    t_xh = sp.dma(d_x, xh[:], x_h[:, :])
                    t_w1 = [pool.dma(d_w[0], w1s[:, k, :], w1[k * P:(k + 1) * P, :]) for k in range(8)]
                    t_st = rms_stats(NT + 1, lambda i: (h[:, i, :] if i < NT else xh[:]), [t_x, t_xh])
                    t_xs_free = [None, None]
                    t_ev = [None, None]
                    for ii in range(NT + 1):
                        i = ii - 1
                        b = ii % 2
                        src = xh[:] if i < 0 else h[:, i, :]
                        ri = NT if i < 0 else i
                        t_n = dve.do(lambda e, src=src, ri=ri, b=b: e.scalar_tensor_tensor(out=xs[b][:], in0=src, scalar=rstd[:, ri:ri + 1], in1=wbc[:], op0=ALU.mult, op1=ALU.mult), [t_st, t_wbc, t_xs_free[b]])
                        tt = None
                        for k in range(8):
                            bank = pb[2 * b + (k // 4)]
                            tt = pe.do(lambda e, k=k, b=b, bank=bank: e.transpose(out=bank[:, (k % 4) * P:(k % 4 + 1) * P], in_=xs[b][:, k * P:(k + 1) * P], identity=ident[:]), [t_n, t_c, t_ev[b]])
                        t_xs_free[b] = tt
                        evs = []
                        for hh in range(2):
                            bank = pb[2 * b + hh]
                            if i < 0:
                                for k4 in range(4):
                                    evs.append(act.do(lambda e, k4=k4, hh=hh, bank=bank: e.activation(out=xT[:, hh * 4 + k4, 0:HAL], in_=bank[:, k4 * P + (P - HAL):(k4 + 1) * P], func=AF.Copy), [tt]))
                            else:
                                c0 = HAL + i * P
                                eng = act if hh == 0 else dve
                                if hh == 0:
                                    evs.append(act.do(lambda e, hh=hh, bank=bank, c0=c0: e.activation(out=xT[:, hh * 4:(hh + 1) * 4, c0:c0 + P], in_=bank[:].rearrange("p (k t) -> p k t", k=4), func=AF.Copy), [tt]))
                                else:
                                    evs.append(dve.do(lambda e, hh=hh, bank=bank, c0=c0: e.tensor_copy(out=xT[:, hh * 4:(hh + 1) * 4, c0:c0 + P], in_=bank[:].rearrange("p (k t) -> p k t", k=4)), [tt]))
                        t_ev[b] = evs
                    t_xT = [t_ev[0], t_ev[1]]
                    NCB = 5
                    CBW = TALL // NCB
                    t_sgfree = [None, None]
                    t_bankfree = [None, None]
                    it = 0
                    t_u = []
                    for cb in range(NCB):
                        for c in range(8):
                            b = it % 2
                            it += 1
                            psA = pb[b]
                            psB = pb[2 + b]
                            tm = None
                            for k in range(8):
                                tm = pe.do(lambda e, k=k, c=c, cb=cb, psA=psA: e.matmul(psA[:, 0:CBW], lhsT=w1s[:, k, c * P:(c + 1) * P], rhs=xT[:, k, cb * CBW:(cb + 1) * CBW], start=(k == 0), stop=(k == 7)), [t_xT, t_w1, t_bankfree[b]])
                            for k in range(8):
                                tm = pe.do(lambda e, k=k, c=c, cb=cb, psB=psB: e.matmul(psB[:, 0:CBW], lhsT=w1s[:, k, D + c * P:D + (c + 1) * P], rhs=xT[:, k, cb * CBW:(cb + 1) * CBW], start=(k == 0), stop=(k == 7)))
                            t_s = act.do(lambda e, c=c, b=b, psB=psB: e.activation(out=sg[b][:, 0:CBW], in_=psB[:, 0:CBW], func=AF.Sigmoid, bias=colv[:, C_B1 + 8 + c:C_B1 + 9 + c]), [tm, t_sgfree[b]])
                            t_g = dve.do(lambda e, c=c, b=b, cb=cb, psA=psA: e.scalar_tensor_tensor(out=uT[:, c, cb * CBW:(cb + 1) * CBW], in0=psA[:, 0:CBW], scalar=colv[:, C_B1 + c:C_B1 + c + 1], in1=sg[b][:, 0:CBW], op0=ALU.add, op1=ALU.mult), [t_s])
                            t_sgfree[b] = t_g
                            t_bankfree[b] = t_g
                            t_u.append(t_g)
                    t_hm = dve.do(lambda e: e.tensor_scalar(out=uT[:, :, 0:HAL], in0=uT[:, :, 0:HAL], scalar1=colv[:, C_HM:C_HM + 1], scalar2=None, op0=ALU.mult), [t_u])
                    self.barrier()
                with ExitStack() as es3:
                    diag = self.sb("diag", [P, 248, P], BF16, es3)
                    w2s = self.sb("w2s", [P, 8, D], BF16, es3)
                    b2s = self.sb("b2s", [1, D], BF16, es3)
                    TB = 256
                    cT = self.sb("cT", [P, 8, TB], F32, es3)
                    csq = self.sb("csq", [P, 2, TB], F32, es3)
                    sT = self.sb("sT", [P, 8, TB], BF16, es3)
                    mu = self.sb("mu", [P, TB], F32, es3)
                    var = self.sb("var", [P, TB], F32, es3)
                    tmp = [self.sb("ctmp%d" % i, [P, TB], F32, es3) for i in range(2)]
                    t_w2 = [pool.dma(d_w[1], w2s[:, k, :], w2[k * P:(k + 1) * P, :]) for k in range(8)]
                    t_b2 = pool.dma(d_w[1], b2s[:], b2row[0:1, :])
                    t_dg = None
                    for c in range(8):
                        for j in range(31):
                            t_dg = dve.do(lambda e, c=c, j=j: e.tensor_scalar(out=diag[:, c * 31 + j, :], in0=identb[:], scalar1=colv[:, C_DWW + c * 31 + j:C_DWW + c * 31 + j + 1], scalar2=None, op0=ALU.mult), [t_idb])
                    t_prev_blk = None
                    t_cbank = [None, None]
                    t_csqfree = [None, None]
                    t_obank = [None, None]
                    oi = 0
                    for tb in range(TOK // TB):
                        t0 = tb * TB
                        psM, psQ = pb[2], pb[3]
                        tm_s = None
                        for c in range(8):
                            b = c % 2
                            psC = pb[b]
                            tm = None
                            for j in range(31):
                                tm = pe.do(lambda e, c=c, j=j, psC=psC, t0=t0: e.matmul(psC[:, 0:TB], lhsT=diag[:, c * 31 + j, :], rhs=uT[:, c, t0 + 2 + j:t0 + 2 + j + TB], start=(j == 0), stop=(j == 30)), [t_dg, t_cbank[b]])
                            ta = act.do(lambda e, c=c, psC=psC: e.activation(out=cT[:, c, :], in_=psC[:, 0:TB], func=AF.Identity, bias=colv[:, C_DWB + c:C_DWB + c + 1]), [tm, t_prev_blk])
                            tq = act.do(lambda e, c=c, b=b, psC=psC: e.activation(out=csq[:, b, :], in_=psC[:, 0:TB], func=AF.Square, bias=colv[:, C_DWB + c:C_DWB + c + 1]), [tm, t_csqfree[b]])
                            tm_s = pe.do(lambda e, c=c: e.matmul(psM[:, 0:TB], lhsT=onesm[:], rhs=cT[:, c, :], start=(c == 0), stop=(c == 7)), [ta, tq, t_o2, t_prev_blk])
                            tm_s = pe.do(lambda e, c=c, b=b: e.matmul(psQ[:, 0:TB], lhsT=onesm[:], rhs=csq[:, b, :], start=(c == 0), stop=(c == 7)))
                            t_csqfree[b] = tm_s
                            t_cbank[b] = tq
                        tm = tm_s
                        t1 = dve.do(lambda e: e.tensor_copy(out=mu[:], in_=psM[:, 0:TB]), [tm, t_prev_blk])
                        t2 = dve.do(lambda e: e.tensor_tensor(out=var[:], in0=mu[:], in1=mu[:], op=ALU.mult), [t1])
                        t3 = dve.do(lambda e: e.tensor_tensor(out=var[:], in0=psQ[:, 0:TB], in1=var[:], op=ALU.subtract), [t2])
                        t4 = dve.do(lambda e: e.tensor_scalar(out=var[:], in0=var[:], scalar1=EPS, scalar2=None, op0=ALU.add), [t3])
                        t5 = act.do(lambda e: e.activation(out=var[:], in_=var[:], func=AF.Sqrt), [t4])
                        t6 = dve.do(lambda e: e.reciprocal(out=var[:], in_=var[:]), [t5])
                        t_s = []
                        t_tmpfree = [None, None]
                        for c in range(8):
                            b = c % 2
                            ta = dve.do(lambda e, c=c, b=b: e.tensor_tensor(out=tmp[b][:], in0=cT[:, c, :], in1=mu[:], op=ALU.subtract), [t6, t_tmpfree[b]])
                            tb2 = dve.do(lambda e, c=c, b=b: e.tensor_tensor(out=tmp[b][:], in0=tmp[b][:], in1=var[:], op=ALU.mult), [ta])
                            tc = act.do(lambda e, c=c, b=b: e.activation(out=sT[:, c, :], in_=tmp[b][:], func=AF.Silu, scale=colv[:, C_LNG + c:C_LNG + c + 1], bias=colv[:, C_LNB + c:C_LNB + c + 1]), [tb2, t_prev_blk])
                            t_tmpfree[b] = tc
                            t_s.append(tc)
                        t_blk = []
                        for ti in range(TB // P):
                            i = t0 // P + ti
                            for hh in range(2):
                                b = oi % 2
                                oi += 1
                                psO = pb[4 + b]
                                tm = None
                                for k in range(8):
                                    tm = pe.do(lambda e, k=k, ti=ti, hh=hh, psO=psO: e.matmul(psO[:], lhsT=sT[:, k, ti * P:(ti + 1) * P], rhs=w2s[:, k, hh * 512:(hh + 1) * 512], start=(k == 0), stop=False), [t_s, t_w2, t_obank[b]])
                                tm = pe.do(lambda e, hh=hh, psO=psO: e.matmul(psO[:], lhsT=onesb[0:1, :], rhs=b2s[0:1, hh * 512:(hh + 1) * 512], start=False, stop=True), [t_b2, t_o1])
                                tr = dve.do(lambda e, i=i, hh=hh, psO=psO: e.tensor_tensor(out=h[:, i, hh * 512:(hh + 1) * 512], in0=psO[:], in1=h[:, i, hh * 512:(hh + 1) * 512], op=ALU.add), [tm])
                                t_obank[b] = tr
                                t_blk.append(tm)
                        t_prev_blk = t_blk + [t6] + t_s
                    self.barrier()

        def moe(l):
            with ExitStack() as em:
                wrs = self.sb("wrs", [P, 8, 36], F32, em)
                rb = self.sb("rb", [P, 36], F32, em)
                ltri = self.sb("ltri", [P, P], BF16, em)
                ltf = self.sb("ltf", [P, P], F32, em)
                ebase = self.sb("ebase", [P, NE], F32, em)
                gates = self.sb("gates", [P, NT, 2], F32, em)
                slots = self.sb("slots", [P, NT, 2], I32, em)
                slotg = self.sb("slotg", [P, NT, 2], I32, em)
                zrow = self.sb("zrow", [8, D], F32, em)
                t_zr = dve.do(lambda e: e.memset(zrow[:], 0.0))
                t_zr = sp.dma(d_o, Yg[NSLOT:NSLOT + 8, :], zrow[:], [t_zr])
                ohs = self.sb("ohs", [P, NT, NE], BF16, em)
                xn = [self.sb("xn%d" % i, [P, D], F32, em) for i in range(2)]
                xnb = [self.sb("xnb%d" % i, [P, D], BF16, em) for i in range(2)]
                xnT = [self.sb("xnT%d" % i, [P, 8, P], F32, em) for i in range(2)]
                def small(name, w, dt=F32):
                    return [self.sb("%s%d" % (name, i), [P, w], dt, em) for i in range(2)]
                lg = small("lg", 36); gmax = small("gmax", 1); ngmax = small("ngmax", 1); gm = small("gm", 4); gex = small("gex", 4)
                gsum = small("gsum", 1); gval = small("gval", 1); pen = small("pen", 4); ml = small("ml", NE); top8 = small("top8", 8)
                oh = [small("oh1_", NE), small("oh2_", NE)]; dd = small("dd", 1); p2 = small("p2", 1)
                ohsf = small("ohsf", NE); pref = small("pref", NE); prod = small("prod", NE)
                rk = small("rk", 2); ebk = small("ebk", 2); ov = small("ov", 2); slf = small("slf", 2); gt = small("gt", 2)

                t_k = [sp.dma(d_c, wbc[:], rows[R_FFN0 + l:R_FFN0 + l + 1, :].partition_broadcast(P)),
                       sp.dma(d_c, wrs[:], wr[l].rearrange("(k p) n -> p k n", p=P)),
                       sp.dma(d_c, rb[:], rbias[l:l + 1, :].partition_broadcast(P)),
                       sp.dma(d_c, ltf[:], ltri_d[:, :]),
                       sp.dma(d_c, ebase[:], ebase_d[:, :])]
                t_lt = dve.do(lambda e: e.tensor_copy(out=ltri[:], in_=ltf[:]), [t_k])
                t_st = rms_stats(NT, lambda i: h[:, i, :], [])
                t_xn_free = [None, None]; t_xnb_free = [None, None]; t_xnT_free = [None, None]
                t_small_free = [None, None]
                t_bank_free = [None, None]
                t_scat = []
                t_ohs = []
                for i in range(NT):
                    b = i % 2
                    t_n = dve.do(lambda e, i=i, b=b: e.scalar_tensor_tensor(out=xn[b][:], in0=h[:, i, :], scalar=rstd[:, i:i + 1], in1=wbc[:], op0=ALU.mult, op1=ALU.mult), [t_st, t_k, t_xn_free[b]])
                    t_nb = act.do(lambda e, b=b: e.activation(out=xnb[b][:], in_=xn[b][:], func=AF.Copy), [t_n, t_xnb_free[b]])
                    tt = None
                    for k in range(8):
                        bank = pb[2 * b + (k // 4)]
                        tt = pe.do(lambda e, k=k, b=b, bank=bank: e.transpose(out=bank[:, (k % 4) * P:(k % 4 + 1) * P], in_=xn[b][:, k * P:(k + 1) * P], identity=ident[:]), [t_n, t_bank_free[b]])
                    te0 = act.do(lambda e, b=b: e.activation(out=xnT[b][:, 0:4, :], in_=pb[2 * b][:].rearrange("p (k t) -> p k t", k=4), func=AF.Copy), [tt, t_xnT_free[b]])
                    te1 = dve.do(lambda e, b=b: e.tensor_copy(out=xnT[b][:, 4:8, :], in_=pb[2 * b + 1][:].rearrange("p (k t) -> p k t", k=4)), [tt, t_xnT_free[b]])
                    t_xn_free[b] = [tt, t_nb]
                    t_bank_free[b] = [te0, te1]
                    psL = pb[4 + b]
                    tm = None
                    for k in range(8):
                        tm = pe.do(lambda e, k=k, b=b, psL=psL: e.matmul(psL[:, 0:36], lhsT=xnT[b][:, k, :], rhs=wrs[:, k, :], start=(k == 0), stop=(k == 7)), [te0, te1, t_k, t_small_free[b]])
                    t_xnT_free[b] = tm
                    V = lambda fn, deps=(): dve.do(fn, deps)
                    t = V(lambda e, b=b, psL=psL: e.tensor_tensor(out=lg[b][:], in0=psL[:, 0:36], in1=rb[:], op=ALU.add), [tm, t_small_free[b]])
                    t = V(lambda e, b=b: e.reduce_max(out=gmax[b][:], in_=lg[b][:, 0:4], axis=AX.X), [t])
                    t = V(lambda e, b=b: e.tensor_scalar(out=gm[b][:], in0=lg[b][:, 0:4], scalar1=gmax[b][:, 0:1], scalar2=None, op0=ALU.is_equal), [t])
                    t = V(lambda e, b=b: e.tensor_scalar(out=ngmax[b][:], in0=gmax[b][:], scalar1=-1.0, scalar2=None, op0=ALU.mult), [t])
                    ta = act.do(lambda e, b=b: e.activation(out=gex[b][:], in_=lg[b][:, 0:4], func=AF.Exp, bias=ngmax[b][:, 0:1], accum_out=gsum[b][:]), [t])
                    t = V(lambda e, b=b: e.reciprocal(out=gval[b][:], in_=gsum[b][:]), [ta])
                    t = V(lambda e, b=b: e.tensor_scalar(out=pen[b][:], in0=gm[b][:], scalar1=1.0, scalar2=1e30, op0=ALU.subtract, op1=ALU.mult), [t])
                    for g in range(4):
                        t = V(lambda e, b=b, g=g: e.tensor_scalar(out=ml[b][:, g * 8:(g + 1) * 8], in0=lg[b][:, 4 + g * 8:4 + (g + 1) * 8], scalar1=pen[b][:, g:g + 1], scalar2=None, op0=ALU.add), [t])
                    t = V(lambda e, b=b: e.max(out=top8[b][:], in_=ml[b][:]), [t])
                    for kk in range(2):
                        t = V(lambda e, b=b, kk=kk: e.tensor_scalar(out=oh[kk][b][:], in0=ml[b][:], scalar1=top8[b][:, kk:kk + 1], scalar2=None, op0=ALU.is_equal), [t])
                    t = V(lambda e, b=b: e.tensor_tensor(out=dd[b][:], in0=top8[b][:, 1:2], in1=top8[b][:, 0:1], op=ALU.subtract), [t])
                    ta = act.do(lambda e, b=b: e.activation(out=p2[b][:], in_=dd[b][:], func=AF.Sigmoid), [t])
                    t = V(lambda e, b=b: e.tensor_tensor(out=gt[b][:, 1:2], in0=gval[b][:], in1=p2[b][:], op=ALU.mult), [ta])
                    t = V(lambda e, b=b: e.tensor_tensor(out=gt[b][:, 0:1], in0=gval[b][:], in1=gt[b][:, 1:2], op=ALU.subtract), [t])
                    t = V(lambda e, b=b: e.tensor_tensor(out=ohsf[b][:], in0=oh[0][b][:], in1=oh[1][b][:], op=ALU.add), [t])
                    t_oh = V(lambda e, b=b, i=i: e.tensor_copy(out=ohs[:, i, :], in_=ohsf[b][:]), [t])
                    t_ohs.append(t_oh)
                    psP = pb[6]
                    tm2 = None
                    for j in range(i):
                        tm2 = pe.do(lambda e, j=j: e.matmul(psP[:, 0:NE], lhsT=onesb[:], rhs=ohs[:, j, :], start=(j == 0), stop=False), [t_ohs[j], t_o1, t_small_free[1 - b] if j == 0 else None])
                    tm2 = pe.do(lambda e, i=i: e.matmul(psP[:, 0:NE], lhsT=ltri[:], rhs=ohs[:, i, :], start=(i == 0), stop=True), [t_oh, t_lt, t_small_free[1 - b] if i == 0 else None])
                    t = V(lambda e, b=b: e.tensor_copy(out=pref[b][:], in_=psP[:, 0:NE]), [tm2])
                    t_pref = t
                    for kk in range(2):
                        t = V(lambda e, b=b, kk=kk: e.tensor_tensor(out=prod[b][:], in0=oh[kk][b][:], in1=pref[b][:], op=ALU.mult), [t])
                        t = V(lambda e, b=b, kk=kk: e.reduce_sum(out=rk[b][:, kk:kk + 1], in_=prod[b][:], axis=AX.X), [t])
                        t = V(lambda e, b=b, kk=kk: e.tensor_tensor(out=prod[b][:], in0=oh[kk][b][:], in1=ebase[:], op=ALU.mult), [t])
                        t = V(lambda e, b=b, kk=kk: e.reduce_sum(out=ebk[b][:, kk:kk + 1], in_=prod[b][:], axis=AX.X), [t])
                    t = V(lambda e, b=b: e.tensor_scalar(out=ov[b][:], in0=rk[b][:], scalar1=float(CAP) - 0.5, scalar2=float(NSLOT), op0=ALU.is_gt, op1=ALU.mult), [t])
                    t = V(lambda e, b=b: e.tensor_tensor(out=slf[b][:], in0=rk[b][:], in1=ebk[b][:], op=ALU.add), [t])
                    t = V(lambda e, b=b: e.tensor_tensor(out=slf[b][:], in0=slf[b][:], in1=ov[b][:], op=ALU.add), [t])
                    t_sl = V(lambda e, b=b, i=i: e.tensor_copy(out=slots[:, i, :], in_=slf[b][:]), [t])
                    t = V(lambda e, b=b: e.tensor_scalar(out=slf[b][:], in0=slf[b][:], scalar1=float(NSLOT), scalar2=None, op0=ALU.min), [t_sl])
                    t_sl2 = V(lambda e, b=b, i=i: e.tensor_copy(out=slotg[:, i, :], in_=slf[b][:]), [t])
                    t = V(lambda e, b=b: e.tensor_scalar(out=ov[b][:], in0=ov[b][:], scalar1=-1.0 / NSLOT, scalar2=1.0, op0=ALU.mult, op1=ALU.add), [t_sl])
                    t_g = V(lambda e, b=b, i=i: e.tensor_tensor(out=gates[:, i, :], in0=gt[b][:], in1=ov[b][:], op=ALU.mult), [t])
                    t_small_free[b] = [t_g, tm2, t_sl2]
                    tsc = []
                    for kk in range(2):
                        tsc.append(pool.raw16(d_sc, lambda e, b=b, i=i, kk=kk: e.indirect_dma_start(out=Xg[:, :], out_offset=bass.IndirectOffsetOnAxis(ap=slots[:, i, kk:kk + 1], axis=0), in_=xnb[b][:], in_offset=None, bounds_check=NSLOT - 1, oob_is_err=False), [t_sl, t_nb]))
                    t_xnb_free[b] = tsc
                    t_scat += tsc
                self.barrier()
                with ExitStack() as ee:
                    NS = CAP // P
                    xg = [self.sb("xg%d" % i, [P, NS, D], BF16, ee) for i in range(2)]
                    xgT = self.sb("xgT", [P, 8, CAP], BF16, ee)
                    wgs = [self.sb("wgs%d" % i, [P, 8, 512], BF16, ee) for i in range(2)]
                    wus = [self.sb("wus%d" % i, [P, 8, 512], BF16, ee) for i in range(2)]
                    wds = [self.sb("wds%d" % i, [P, 4, D], BF16, ee) for i in range(2)]
                    aT = self.sb("aT", [P, 4, CAP], BF16, ee)
                    sgl = [self.sb("sgl%d" % i, [P, CAP], F32, ee) for i in range(2)]
                    ysb = [self.sb("ysb%d" % i, [P, D], F32, ee) for i in range(2)]
                    t_wfree = [None, None]; t_xgfree = [None, None]
                    t_xgT_free = None; t_aT_free = None
                    t_ysb_free = [None, None]; t_sgl_free = [None, None]
                    t_bank = {}
                    t_ystore = []

                    def issue_loads(e_):
                        b = e_ % 2
                        tw = [pool.dma(d_w[b], wgs[b][:], wg[self.moe_layers.index(l), e_].rearrange("(k p) n -> p k n", p=P), [t_wfree[b]]),
                              pool.dma(d_w[b], wus[b][:], wu[self.moe_layers.index(l), e_].rearrange("(k p) n -> p k n", p=P)),
                              pool.dma(d_w[b], wds[b][:], wd[self.moe_layers.index(l), e_].rearrange("(k p) n -> p k n", p=P))]
                        tx = sp.dma(d_xg[b], xg[b][:], Xg[e_ * CAP:(e_ + 1) * CAP, :].rearrange("(s p) d -> p s d", p=P), [t_xgfree[b]])
                        return tw, tx

                    nxt = issue_loads(0)
                    yi = 0
                    for e_ in range(NE):
                        b = e_ % 2
                        tw, tx = nxt
                        if e_ + 1 < NE:
                            nxt = issue_loads(e_ + 1)
                        tev = []
                        for k in range(8):
                            tt = None
                            for s in range(NS):
                                tt = pe.do(lambda e, k=k, s=s, b=b: e.transpose(out=pbb[:, s * P:(s + 1) * P], in_=xg[b][:, s, k * P:(k + 1) * P], identity=identb[:]), [tx, t_idb, t_bank.get("bb")])
                            if k % 2 == 0:
                                tv = act.do(lambda e, k=k: e.activation(out=xgT[:, k, :], in_=pbb[:, 0:CAP], func=AF.Copy), [tt, t_xgT_free])
                            else:
                                tv = dve.do(lambda e, k=k: e.tensor_copy(out=xgT[:, k, :], in_=pbb[:, 0:CAP]), [tt, t_xgT_free])
                            t_bank["bb"] = tv
                            tev.append(tv)
                        t_xgfree[b] = tt
                        t_a = []
                        tmm = None
                        for fc in range(4):
                            pg = pb[(fc % 2) * 2]
                            pu = pb[(fc % 2) * 2 + 1]
                            bf_ = fc % 2
                            for k in range(8):
                                tmm = pe.do(lambda e, k=k, fc=fc, b=b, pg=pg: e.matmul(pg[:, 0:CAP], lhsT=wgs[b][:, k, fc * P:(fc + 1) * P], rhs=xgT[:, k, :], start=(k == 0), stop=(k == 7)), [tev, tw, t_bank.get(("g", bf_))])
                            for k in range(8):
                                tmm = pe.do(lambda e, k=k, fc=fc, b=b, pu=pu: e.matmul(pu[:, 0:CAP], lhsT=wus[b][:, k, fc * P:(fc + 1) * P], rhs=xgT[:, k, :], start=(k == 0), stop=(k == 7)))
                            ts = act.do(lambda e, pg=pg, bf_=bf_: e.activation(out=sgl[bf_][:], in_=pg[:, 0:CAP], func=AF.Silu), [tmm, t_sgl_free[bf_]])
                            ta = dve.do(lambda e, fc=fc, pu=pu, bf_=bf_: e.tensor_tensor(out=aT[:, fc, :], in0=sgl[bf_][:], in1=pu[:, 0:CAP], op=ALU.mult), [ts, t_aT_free])
                            t_sgl_free[bf_] = ta
                            t_bank[("g", bf_)] = ta
                            t_a.append(ta)
                        t_xgT_free = tmm
                        tlast = None
                        for s in range(NS):
                            yb = yi % 2
                            yi += 1
                            tys = []
                            for hh in range(2):
                                py = pb[4 + hh]
                                for fc in range(4):
                                    tlast = pe.do(lambda e, fc=fc, s=s, hh=hh, b=b, py=py: e.matmul(py[:], lhsT=aT[:, fc, s * P:(s + 1) * P], rhs=wds[b][:, fc, hh * 512:(hh + 1) * 512], start=(fc == 0), stop=(fc == 3)), [t_a, tw, t_bank.get(("y", hh))])
                                if hh == 0:
                                    tv = act.do(lambda e, yb=yb, py=py: e.activation(out=ysb[yb][:, 0:512], in_=py[:], func=AF.Copy), [tlast, t_ysb_free[yb]])
                                else:
                                    tv = dve.do(lambda e, yb=yb, py=py: e.tensor_copy(out=ysb[yb][:, 512:1024], in_=py[:]), [tlast, t_ysb_free[yb]])
                                t_bank[("y", hh)] = tv
                                tys.append(tv)
                            r0 = e_ * CAP + s * P
                            tst = sp.dma(d_y[yb], Yg[r0:r0 + P, :], ysb[yb][:], tys)
                            t_ysb_free[yb] = tst
                            t_ystore.append(tst)
                        t_aT_free = tlast
                        t_wfree[b] = tlast
                    self.barrier()
                with ExitStack() as ec:
                    yg = [self.sb("yg%d" % i, [P, D], F32, ec) for i in range(4)]
                    t_z = [dve.do(lambda e, i=i: e.memset(yg[i][:], 0.0)) for i in range(4)]
                    t_ygfree = [t_z[i] for i in range(4)]
                    gi = 0
                    for i in range(NT):
                        for kk in range(2):
                            g_ = gi % 4
                            gi += 1
                            tg = pool.raw16(d_ga[g_], lambda e, g_=g_, i=i, kk=kk: e.indirect_dma_start(out=yg[g_][:], out_offset=None, in_=Yg[:, :], in_offset=bass.IndirectOffsetOnAxis(ap=slotg[:, i, kk:kk + 1], axis=0)), [t_ygfree[g_]])
                            tr = dve.do(lambda e, g_=g_, i=i, kk=kk: e.scalar_tensor_tensor(out=h[:, i, :], in0=yg[g_][:], scalar=gates[:, i, kk:kk + 1], in1=h[:, i, :], op0=ALU.mult, op1=ALU.add), [tg])
                            t_ygfree[g_] = tr
                    self.barrier()

        if "moe0" in st:
            moe(0)

        def hgrn(full):
            with ExitStack() as eh:
                onT = self.sb("onT", [P, 8, TOK], BF16, eh) if full else None
                S = self.sb("S", [P, 8, P], F32, eh)
                with ExitStack() as eh2:
                    xT2 = self.sb("xT2", [P, 8, TOK], BF16, eh2)
                    lbrow = self.sb("lbrow", [P, D], F32, eh2)
                    omlrow = self.sb("omlrow", [P, D], F32, eh2)
                    lbc = self.sb("lbc", [P, 8], F32, eh2)
                    omlc = self.sb("omlc", [P, 8], F32, eh2)
                    bdle = self.sb("bdle_s", [P, P], F32, eh2)
                    bu = self.sb("bu_s", [P, P], F32, eh2)
                    ones128 = self.sb("ones128", [P, P], F32, eh2)
                    Sb = self.sb("Sb", [P, P], BF16, eh2)
                    wh = [self.sb("wh%d" % i, [P, 8, 4, P], BF16, eh2) for i in range(2)]
                    xs = [self.sb("hxs%d" % i, [P, D], F32, eh2) for i in range(2)]

                    def T2(name, dt=F32, w=P):
                        return [self.sb("%s%d" % (name, i), [P, w], dt, eh2) for i in range(2)]
                    sgt = T2("sgt"); fgt = T2("fgt"); lft = T2("lft"); ktok = T2("ktok"); eRt = T2("eRt"); sqt = T2("sqt")
                    eGt = T2("eGt"); nGt = T2("nGt"); skt = T2("skt"); osqt = T2("osqt"); rst = T2("rst"); tmpo = T2("tmpo")
                    vtok = T2("vtok", BF16); khat = T2("khat", BF16); qT = T2("qT", BF16); kT = T2("kT", BF16); scT = T2("scT", BF16)
                    sgt2 = T2("sgt2"); dec = T2("dec", F32, 2)

                    t_k = [sp.dma(d_c, wbc[:], rows[R_HGN:R_HGN + 1, :].partition_broadcast(P)),
                           sp.dma(d_c, lbrow[:], rows[5:6, :].partition_broadcast(P)),
                           sp.dma(d_c, omlrow[:], rows[6:7, :].partition_broadcast(P)),
                           sp.dma(d_c, bdle[:], bdle_d[:, :]),
                           sp.dma(d_c, bu[:], bu_d[:, :])]
                    t = dve.do(lambda e: e.tensor_tensor(out=lbrow[:], in0=omlrow[:], in1=lbrow[:], op=ALU.subtract), [t_k])
                    t = act.do(lambda e: e.activation(out=lbrow[:], in_=lbrow[:], func=AF.Sigmoid), [t])
                    t_lbr = dve.do(lambda e: e.tensor_scalar(out=omlrow[:], in0=lbrow[:], scalar1=-1.0, scalar2=1.0, op0=ALU.mult, op1=ALU.add), [t])
                    t = dve.do(lambda e: e.tensor_tensor(out=lbc[:], in0=colv[:, C_LB1:C_LB1 + 8], in1=colv[:, C_LB0:C_LB0 + 8], op=ALU.subtract), [t_c])
                    t = act.do(lambda e: e.activation(out=lbc[:], in_=lbc[:], func=AF.Sigmoid), [t])
                    t_lbc = dve.do(lambda e: e.tensor_scalar(out=omlc[:], in0=lbc[:], scalar1=-1.0, scalar2=1.0, op0=ALU.mult, op1=ALU.add), [t])
                    t_on = dve.do(lambda e: e.memset(ones128[:], 1.0 / P))
                    if full:
                        t_si = [sp.dma(d_s, S[:, hd, :], s_init[hd]) for hd in range(8)]
                    else:
                        t_si = [dve.do(lambda e: e.memset(S[:], 0.0))]
                    t_st = rms_stats(NT, lambda i: h[:, i, :], [])
                    t_xs_free = [None, None]
                    t_ev = [None, None]
                    for i in range(NT):
                        b = i % 2
                        t_n = dve.do(lambda e, i=i, b=b: e.scalar_tensor_tensor(out=xs[b][:], in0=h[:, i, :], scalar=rstd[:, i:i + 1], in1=wbc[:], op0=ALU.mult, op1=ALU.mult), [t_st, t_k, t_xs_free[b]])
                        tt = None
                        for k in range(8):
                            bank = pb[2 * b + (k // 4)]
                            tt = pe.do(lambda e, k=k, b=b, bank=bank: e.transpose(out=bank[:, (k % 4) * P:(k % 4 + 1) * P], in_=xs[b][:, k * P:(k + 1) * P], identity=ident[:]), [t_n, t_ev[b]])
                        t_xs_free[b] = tt
                        c0 = i * P
                        e0 = act.do(lambda e, b=b, c0=c0: e.activation(out=xT2[:, 0:4, c0:c0 + P], in_=pb[2 * b][:].rearrange("p (k t) -> p k t", k=4), func=AF.Copy), [tt])
                        e1 = dve.do(lambda e, b=b, c0=c0: e.tensor_copy(out=xT2[:, 4:8, c0:c0 + P], in_=pb[2 * b + 1][:].rearrange("p (k t) -> p k t", k=4)), [tt])
                        t_ev[b] = [e0, e1]
                    self.barrier()

                    t_whfree = [None, None]

                    def load_wh(hd):
                        b = hd % 2
                        tl = []
                        for part in range(4):
                            c0 = part * D + hd * P
                            tl.append(pool.dma(d_wh[b], wh[b][:, :, part, :], w_in[:, c0:c0 + P].rearrange("(k p) n -> p k n", p=P), [t_whfree[b]]))
                        return tl

                    snaps = {}
                    bf = {}
                    it = 0
                    nxt = load_wh(0)
                    t_inter = None
                    t_Supd = None
                    hot = [act, dve, pe]
                    for hd in range(8):
                        wb = hd % 2
                        t_wh = nxt
                        if hd + 1 < 8:
                            nxt = load_wh(hd + 1)
                        hs = slice(hd * P, (hd + 1) * P)
                        t_Sb = None
                        if full:
                            t_Sb = act.do(lambda e, hd=hd: e.activation(out=Sb[:], in_=S[:, hd, :], func=AF.Copy), [t_si, t_inter])
                        t_Supd = t_si
                        tlast_pe = None
                        for i in range(NT):
                            b = it % 2
                            if it - 2 in snaps:
                                for en in hot:
                                    en.wait(snaps[it - 2])
                            pT = pb[0][:, 0:256]; pFq = pb[0][:, 256:384]; pFf = pb[0][:, 384:512]
                            pFg = pb[1][:, 0:P]
                            psR = pb[2][:, 0:P]; psG = pb[2][:, P:2 * P]
                            psS = pb[3][:, 0:P]; psO = pb[4][:, 0:P]; psD = pb[5][:, 0:P]; psV = pb[6][:, 0:P]
                            cs = slice(i * P, (i + 1) * P)
                            tm = None
                            for k in range(8):
                                tm = pe.do(lambda e, k=k, wb=wb, cs=cs, pT=pT: e.matmul(pT, lhsT=xT2[:, k, cs], rhs=wh[wb][:, k, 1:3, :].rearrange("p a n -> p (a n)"), start=(k == 0), stop=(k == 7)), [t_wh, bf.get(0)])
                            if full:
                                for dst, part in ((pFq, 0), (pFf, 1)):
                                    for k in range(8):
                                        tm = pe.do(lambda e, k=k, wb=wb, cs=cs, dst=dst, part=part: e.matmul(dst, lhsT=wh[wb][:, k, part, :], rhs=xT2[:, k, cs], start=(k == 0), stop=(k == 7)))
                                tmFg = None
                                for k in range(8):
                                    tmFg = pe.do(lambda e, k=k, wb=wb, cs=cs, pFg=pFg: e.matmul(pFg, lhsT=wh[wb][:, k, 3, :], rhs=xT2[:, k, cs], start=(k == 0), stop=(k == 7)), [bf.get(1)])
                            t_sg = act.do(lambda e, b=b, pT=pT: e.activation(out=sgt[b][:], in_=pT[:, 0:P], func=AF.Sigmoid), [tm])
                            t1 = dve.do(lambda e, b=b, hs=hs: e.tensor_tensor(out=fgt[b][:], in0=sgt[b][:], in1=omlrow[:, hs], op=ALU.mult), [t_sg, t_lbr])
                            t1 = dve.do(lambda e, b=b, hs=hs: e.tensor_tensor(out=fgt[b][:], in0=fgt[b][:], in1=lbrow[:, hs], op=ALU.add), [t1])
                            t_lf = act.do(lambda e, b=b: e.activation(out=lft[b][:], in_=fgt[b][:], func=AF.Ln), [t1])
                            t_vt = dve.do(lambda e, b=b, pT=pT: e.tensor_copy(out=vtok[b][:], in_=pT[:, P:2 * P]), [tm])
                            bf[0] = [t_sg, t_vt]
                            t2 = dve.do(lambda e, b=b: e.tensor_scalar(out=ktok[b][:], in0=sgt[b][:], scalar1=-1.0, scalar2=1.0, op0=ALU.mult, op1=ALU.add), [t_sg])
                            t_kt = dve.do(lambda e, b=b, hs=hs: e.tensor_tensor(out=ktok[b][:], in0=ktok[b][:], in1=omlrow[:, hs], op=ALU.mult), [t2])
                            tmR = pe.do(lambda e, b=b, psR=psR: e.matmul(psR, lhsT=bu[:], rhs=lft[b][:], start=True, stop=True), [t_lf, t_k, bf.get(2)])
                            tmG = pe.do(lambda e, b=b, psG=psG: e.matmul(psG, lhsT=lft[b][:], rhs=bdle[:], start=True, stop=True))
                            t_eR = act.do(lambda e, b=b, psR=psR: e.activation(out=eRt[b][:], in_=psR, func=AF.Exp), [tmG])
                            t_kh = dve.do(lambda e, b=b: e.tensor_tensor(out=khat[b][:], in0=ktok[b][:], in1=eRt[b][:], op=ALU.mult), [t_eR, t_kt])
                            t_d0 = act.do(lambda e, b=b, psG=psG: e.activation(out=dec[b][:, 0:1], in_=psG[:, 63:64], func=AF.Exp), [tmG])
                            t_dec = act.do(lambda e, b=b, psG=psG: e.activation(out=dec[b][:, 1:2], in_=psG[:, 127:128], func=AF.Exp), [tmG])
                            bf[2] = [t_eR, t_dec]
                            if full:
                                t_sq = act.do(lambda e, b=b, pFq=pFq: e.activation(out=sqt[b][:], in_=pFq, func=AF.Silu), [tm])
                                t_eG = act.do(lambda e, b=b, psG=psG: e.activation(out=eGt[b][:], in_=psG, func=AF.Exp), [tmG])
                                t_q = dve.do(lambda e, b=b: e.tensor_tensor(out=qT[b][:], in0=sqt[b][:], in1=eGt[b][:], op=ALU.mult), [t_sq, t_eG])
                                t_ng = dve.do(lambda e, b=b, psG=psG: e.tensor_scalar(out=nGt[b][:], in0=psG, scalar1=-1.0, scalar2=80.0, op0=ALU.mult, op1=ALU.min), [tmG])
                                t_eng = act.do(lambda e, b=b: e.activation(out=nGt[b][:], in_=nGt[b][:], func=AF.Exp), [t_ng])
                                t_sk = act.do(lambda e, b=b, pFf=pFf: e.activation(out=skt[b][:], in_=pFf, func=AF.Sigmoid, scale=-1.0), [tm])
                                bf[0] = [t_sg, t_vt, t_sq, t_sk]
                                bf[2] = [t_eR, t_dec, t_eG, t_ng]
                                t_kT = dve.do(lambda e, b=b, hd=hd: e.scalar_tensor_tensor(out=kT[b][:], in0=skt[b][:], scalar=omlc[:, hd:hd + 1], in1=nGt[b][:], op0=ALU.mult, op1=ALU.mult), [t_sk, t_eng, t_lbc])
                                t_gg = act.do(lambda e, b=b, pFg=pFg: e.activation(out=sgt2[b][:], in_=pFg, func=AF.Silu), [tmFg])
                                bf[1] = [t_gg]
                                tmS = pe.do(lambda e, b=b, psS=psS: e.matmul(psS, lhsT=kT[b][:], rhs=qT[b][:], start=True, stop=True), [t_kT, t_q, bf.get(3)])
                                t_sc = dve.do(lambda e, b=b, psS=psS: e.tensor_tensor(out=scT[b][:], in0=psS, in1=bdle[:], op=ALU.mult), [tmS])
                                bf[3] = [t_sc]
                            for cc in range(2):
                                r = 64 * cc
                                if full:
                                    t_inter = pe.do(lambda e, b=b, r=r, psO=psO: e.matmul(psO[:, r:r + 64], lhsT=Sb[:], rhs=qT[b][:, r:r + 64], start=True, stop=False), [t_Sb, t_q, bf.get(4)])
                                    t_intra = pe.do(lambda e, b=b, r=r, psO=psO: e.matmul(psO[:, r:r + 64], lhsT=vtok[b][r:r + 64, :], rhs=scT[b][r:r + 64, r:r + 64], start=False, stop=True), [t_sc, t_vt])
                                tmD = pe.do(lambda e, b=b, r=r, psD=psD: e.matmul(psD, lhsT=khat[b][r:r + 64, :], rhs=vtok[b][r:r + 64, :], start=True, stop=True), [t_kh, t_vt, t_Supd])
                                tlast_pe = tmD
                                t_Supd = dve.do(lambda e, b=b, hd=hd, cc=cc, psD=psD: e.scalar_tensor_tensor(out=S[:, hd, :], in0=S[:, hd, :], scalar=dec[b][:, cc:cc + 1], in1=psD, op0=ALU.mult, op1=ALU.add), [tmD, t_dec, t_Supd])
                                if full:
                                    t_Sb = act.do(lambda e, hd=hd: e.activation(out=Sb[:], in_=S[:, hd, :], func=AF.Copy), [t_Supd, t_inter])
                            if full:
                                t_os = act.do(lambda e, b=b, psO=psO: e.activation(out=osqt[b][:], in_=psO, func=AF.Square), [t_intra])
                                tmV = pe.do(lambda e, b=b, psV=psV: e.matmul(psV, lhsT=ones128[:], rhs=osqt[b][:], start=True, stop=True), [t_os, t_on, bf.get(6)])
                                tlast_pe = tmV
                                t3 = dve.do(lambda e, b=b, psV=psV: e.tensor_scalar(out=rst[b][:], in0=psV, scalar1=EPS, scalar2=None, op0=ALU.add), [tmV])
                                bf[6] = [t3]
                                t3 = act.do(lambda e, b=b: e.activation(out=rst[b][:], in_=rst[b][:], func=AF.Sqrt), [t3])
                                t3 = dve.do(lambda e, b=b: e.reciprocal(out=rst[b][:], in_=rst[b][:]), [t3])
                                t3 = dve.do(lambda e, b=b, psO=psO: e.scalar_tensor_tensor(out=tmpo[b][:], in0=psO, scalar=colv[:, C_GN:C_GN + 1], in1=rst[b][:], op0=ALU.mult, op1=ALU.mult), [t3])
                                bf[4] = [t_os, t3]
                                t3 = dve.do(lambda e, b=b, hd=hd, cs=cs: e.tensor_tensor(out=onT[:, hd, cs], in0=tmpo[b][:], in1=sgt2[b][:], op=ALU.mult), [t3, t_gg])
                            snaps[it] = [(en, en.n) for en in hot]
                            it += 1
                        t_whfree[wb] = tlast_pe
                        t_si = t_Supd
                    self.barrier()
                    t_se = [sp.dma(d_s, s_end[hd], S[:, hd, :]) for hd in range(8)]
                    self.t_send = t_se
                if full:
                    with ExitStack() as eo:
                        wo = self.sb("wo", [P, 8, D], BF16, eo)
                        t_wo = pool.dma(d_wh[0], wo[:], w_out.rearrange("(k p) n -> p k n", p=P))
                        t_ob = [None, None]
                        oi = 0
                        for i in range(NT):
                            cs = slice(i * P, (i + 1) * P)
                            for hh in range(2):
                                b = oi % 2
                                oi += 1
                                psO = pb[b]
                                tm = None
                                for hd in range(8):
                                    tm = pe.do(lambda e, hd=hd, cs=cs, hh=hh, psO=psO: e.matmul(psO[:], lhsT=onT[:, hd, cs], rhs=wo[:, hd, hh * 512:(hh + 1) * 512], start=(hd == 0), stop=(hd == 7)), [t_wo, t_ob[b]])
                                t_ob[b] = dve.do(lambda e, i=i, hh=hh, psO=psO: e.tensor_tensor(out=h[:, i, hh * 512:(hh + 1) * 512], in0=psO[:], in1=h[:, i, hh * 512:(hh + 1) * 512], op=ALU.add), [tm])
                        self.barrier()

        self.t_send = []
        if "hgrn_state" in st:
            hgrn(False)
        if "hgrn" in st:
            hgrn(True)

        if "moe1" in st:
            moe(1)

        with ExitStack() as ef:
            ob = [self.sb("ob%d" % i, [P, D], F32, ef) for i in range(2)]
            t_obfree = [None, None]
            t_out = []
            if "final" in st:
                t_wf = sp.dma(d_c, wbc[:], rows[R_FIN:R_FIN + 1, :].partition_broadcast(P))
                t_st = rms_stats(NT, lambda i: h[:, i, :], [])
            for i in range(NT):
                b = i % 2
                if "final" in st:
                    tn = dve.do(lambda e, i=i, b=b: e.scalar_tensor_tensor(out=ob[b][:], in0=h[:, i, :], scalar=rstd[:, i:i + 1], in1=wbc[:], op0=ALU.mult, op1=ALU.mult), [t_st, t_wf, t_obfree[b]])
                    ts = sp.dma(d_o, out_d[i * P:(i + 1) * P, :], ob[b][:], [tn])
                    t_obfree[b] = ts
                else:
                    ts = sp.dma(d_o, out_d[i * P:(i + 1) * P, :], h[:, i, :], [t_x] + [(e, e.n) for e in self.engs if e.n > 0])
                t_out.append(ts)
            sp.wait(t_out, self.t_send)
            self.barrier()

        with nc.Block() as block:
            @block.sync
            def _(e): sp.replay(e)

            @block.scalar
            def _(e): act.replay(e)

            @block.vector
            def _(e): dve.replay(e)

            @block.tensor
            def _(e): pe.replay(e)

            @block.gpsimd
            def _(e): pool.replay(e)
        self.es.close()
        return nc


def host_inputs(inputs, core):
    f = np.float32
    x = np.asarray(inputs["x"], f).reshape(-1, D)
    t0 = core * TOK
    m = {}
    m["x_c"] = np.ascontiguousarray(x[t0:t0 + TOK])
    first = (core % 2 == 0)
    m["x_h"] = np.zeros((P, D), f) if first else np.ascontiguousarray(x[t0 - P:t0])
    cols = np.zeros((P, 320), f)
    cols[:, 0:16] = np.asarray(inputs["conv_pw1_b"], f)[0].reshape(16, P).T
    cols[:, 16:24] = np.asarray(inputs["conv_dw_b"], f)[0].reshape(8, P).T
    cols[:, 24:32] = np.asarray(inputs["conv_ln_g"], f)[0].reshape(8, P).T
    cols[:, 32:40] = np.asarray(inputs["conv_ln_b"], f)[0].reshape(8, P).T
    dw = np.asarray(inputs["conv_dw_w"], f)[0]
    cols[:, 40:288] = dw.reshape(31, 8, P).transpose(2, 1, 0).reshape(P, 248)
    cols[:, 288] = 0.0 if first else 1.0
    lbs = np.asarray(inputs["lower_bounds"], f)
    cols[:, 289:297] = lbs[0].reshape(8, P).T
    cols[:, 297:305] = lbs[1].reshape(8, P).T
    cols[:, 305] = np.asarray(inputs["hgrn_gnorm_w"], f)[0]
    m["cols"] = cols
    rows = np.zeros((8, D), f)
    rows[0] = np.asarray(inputs["conv_norm_w"], f)[0]
    rows[1] = np.asarray(inputs["ffn_norm_w"], f)[0]
    rows[2] = np.asarray(inputs["ffn_norm_w"], f)[1]
    rows[3] = np.asarray(inputs["hgrn_norm_w"], f)[0]
    rows[4] = np.asarray(inputs["final_norm_w"], f)
    rows[5] = lbs[0]
    rows[6] = lbs[1]
    m["rows"] = rows
    m["ident"] = np.eye(P, dtype=f)
    m["ltri"] = np.triu(np.ones((P, P), f), 1)
    m["ebase"] = np.tile((np.arange(NE, dtype=f) * CAP)[None, :], (P, 1))
    ii = np.arange(P)
    same = (ii[:, None] // 64) == (ii[None, :] // 64)
    m["bdle"] = (same & (ii[:, None] <= ii[None, :])).astype(f)
    m["bu"] = (same & (ii[:, None] > ii[None, :])).astype(f)
    m["hgrn_w_in"] = np.asarray(inputs["hgrn_w_in"], f)[0]
    m["hgrn_w_out"] = np.asarray(inputs["hgrn_w_out"], f)[0]
    m["s_init"] = np.zeros((8, P, P), f)
    m["conv_pw1_w"] = np.asarray(inputs["conv_pw1_w"], f)[0]
    m["conv_pw2_w"] = np.asarray(inputs["conv_pw2_w"], f)[0]
    m["conv_pw2_b"] = np.asarray(inputs["conv_pw2_b"], f)[0:1]
    m["wr"] = np.concatenate([np.asarray(inputs["router_grp_w"], f), np.asarray(inputs["router_exp_w"], f).reshape(2, D, 32)], axis=2)
    m["rbias"] = np.concatenate([np.asarray(inputs["router_grp_b"], f), np.asarray(inputs["router_exp_b"], f).reshape(2, 32)], axis=1)
    return m


def run(inputs, stages, x_override=None, s_init=None):
    kb = K(stages)
    nc = kb.build()
    layers = kb.moe_layers or [0]
    f = np.float32
    wgs = np.ascontiguousarray(np.asarray(inputs["moe_w_gate"], f)[layers])
    wus = np.ascontiguousarray(np.asarray(inputs["moe_w_up"], f)[layers])
    wds = np.ascontiguousarray(np.asarray(inputs["moe_w_down"], f)[layers])
    in_maps = []
    for c in range(NCORES):
        m = host_inputs(inputs, c)
        m["moe_w_gate"], m["moe_w_up"], m["moe_w_down"] = wgs, wus, wds
        if x_override is not None:
            m["x_c"] = np.ascontiguousarray(np.asarray(x_override, f).reshape(-1, D)[c * TOK:(c + 1) * TOK])
        if s_init is not None:
            m["s_init"] = np.ascontiguousarray(s_init[c])
        in_maps.append({k: m[k] for k in kb.inp})
    res = run_bass_kernel_spmd(nc, in_maps, core_ids=list(range(NCORES)))
    out = np.concatenate([res.results[c]["out"] for c in range(NCORES)], axis=0)
    s_end = np.stack([res.results[c]["s_end"] for c in range(NCORES)], axis=0)
    return out.reshape(4, 4096, D), s_end


def kernel(**inputs):
    h1, s_end = run(inputs, ("conv", "moe0", "hgrn_state"))
    s_init = np.zeros_like(s_end)
    s_init[1::2] = s_end[0::2]
    out, _ = run(inputs, ("hgrn", "moe1", "final"), x_override=h1, s_init=s_init)
    return out
```
